# Optimizing a Trainium2 kernel written in Bass

```python
import jax
import jax.numpy as jnp
from jax import lax
import numpy as np


D_MODEL = 1024
BATCH = 2
SEQ = 8192
DEPTH = 4

HEAD_DIM = 64
RET_HEADS = 4
DSA_HEADS = 4
DIL_HEADS = 8
RET_W = RET_HEADS * HEAD_DIM
DSA_W = DSA_HEADS * HEAD_DIM
DIL_W = DIL_HEADS * HEAD_DIM
D_MIX = RET_W + DSA_W + DIL_W
RET_CHUNK = 128
RET_ROPE_BASE = 10000.0
KV_LATENT = 128
IDX_HEADS = 8
IDX_DIM = 64
TOPK_MAX = 256
QUERY_BLOCK = 128
DIL_PATTERNS = ((128, 1), (512, 4), (2048, 16))
D_FF = 4 * D_MODEL
NORM_EPS = 1e-6
RET_COLS = 4 * RET_W
DSA_SIZES = (DSA_W, KV_LATENT, IDX_HEADS * IDX_DIM, IDX_DIM, IDX_HEADS)
DSA_COLS = sum(DSA_SIZES)
DIL_COLS = 3 * DIL_W
N_IN = RET_COLS + DSA_COLS + DIL_COLS

kernel_name = 'hymba_style_retention_dsa_dilated_hybrid'


def _offsets(sizes):
    return [int(o) for o in np.cumsum(sizes)[:-1]]


def rms_norm(x, w):
    xf = x.astype(jnp.float32)
    y = xf * lax.rsqrt(jnp.mean(xf * xf, axis=-1, keepdims=True) + NORM_EPS)
    return (y * w.astype(jnp.float32)).astype(x.dtype)


def rotary(x, pos):
    half = x.shape[-1] // 2
    inv = RET_ROPE_BASE ** (-jnp.arange(half, dtype=jnp.float32) / half)
    ang = pos.astype(jnp.float32)[:, None] * inv[None, :]
    cos = jnp.cos(ang)[None, :, None, :]
    sin = jnp.sin(ang)[None, :, None, :]
    xf = x.astype(jnp.float32)
    x1, x2 = xf[..., :half], xf[..., half:]
    return jnp.concatenate([x1 * cos - x2 * sin, x2 * cos + x1 * sin], axis=-1)


def retention_chunkwise(q, k, v):
    B, S, H, d = q.shape
    C = RET_CHUNK
    N = S // C
    log_g = jnp.log(1.0 - 2.0 ** (-5.0 - jnp.arange(H, dtype=jnp.float32)))

    def chunks(t):
        return t.reshape(B, N, C, H, d).transpose(0, 3, 1, 2, 4)

    qc, kc, vc = chunks(q), chunks(k * (d ** -0.5)), chunks(v)
    i = jnp.arange(C, dtype=jnp.float32)
    diff = i[:, None] - i[None, :]
    decay = jnp.exp(jnp.maximum(diff, 0.0)[None] * log_g[:, None, None]) * (diff >= 0)[None]
    inner = jnp.einsum('bhncd,bhnmd->bhncm', qc, kc) * decay[None, :, None]
    inner = jnp.einsum('bhncm,bhnme->bhnce', inner, vc)
    zeta = jnp.exp((C - 1.0 - i)[None, :] * log_g[:, None])
    xi = jnp.exp((i + 1.0)[None, :] * log_g[:, None])
    kv = jnp.einsum('bhnmd,bhnme->nbhde', kc * zeta[None, :, None, :, None], vc)
    g_chunk = jnp.exp(C * log_g)[None, :, None, None]

    def step(state, kv_n):
        return g_chunk * state + kv_n, state

    _, state_prev = lax.scan(step, jnp.zeros((B, H, d, d), jnp.float32), kv)
    cross = jnp.einsum('bhncd,nbhde->bhnce', qc, state_prev) * xi[None, :, None, :, None]
    o = inner + cross
    return o.transpose(0, 2, 3, 1, 4).reshape(B, S, H, d)


def retention_mixer(cols, norm_w, pos):
    B, S, _ = cols.shape
    q, k, v, g = jnp.split(cols, 4, axis=-1)
    shp = (B, S, RET_HEADS, HEAD_DIM)
    o = retention_chunkwise(rotary(q.reshape(shp), pos), rotary(k.reshape(shp), pos),
                            v.reshape(shp).astype(jnp.float32))
    mu = jnp.mean(o, axis=-1, keepdims=True)
    var = jnp.mean(jnp.square(o - mu), axis=-1, keepdims=True)
    o = (o - mu) * lax.rsqrt(var + NORM_EPS) * norm_w.astype(jnp.float32)
    o = o.reshape(B, S, RET_W)
    return (jax.nn.silu(g.astype(jnp.float32)) * o).astype(cols.dtype)


def dsa_mixer(cols, kv_norm_w, w_uk, w_uv):
    B, S, _ = cols.shape
    q, c_kv, q_idx, k_idx, w_idx = jnp.split(cols, _offsets(DSA_SIZES), axis=-1)
    q = q.reshape(B, S, DSA_HEADS, HEAD_DIM)
    c_kv = rms_norm(c_kv, kv_norm_w)
    k = (c_kv @ w_uk).reshape(B, S, DSA_HEADS, HEAD_DIM)
    v = (c_kv @ w_uv).reshape(B, S, DSA_HEADS, HEAD_DIM)
    q_idx = q_idx.reshape(B, S, IDX_HEADS, IDX_DIM)
    w_idx = w_idx * (IDX_HEADS ** -0.5 * IDX_DIM ** -0.5)
    n_sel = min(TOPK_MAX, S // 4)
    nblk = S // QUERY_BLOCK
    key_pos = jnp.arange(S)

    def to_blocks(t):
        return jnp.moveaxis(t.reshape((B, nblk, QUERY_BLOCK) + t.shape[2:]), 1, 0)

    def block(args):
        qb, qib, wb, start = args
        qpos = start + jnp.arange(QUERY_BLOCK)
        logits = jnp.einsum('bqhd,bsd->bqhs', qib, k_idx)
        score = jnp.einsum('bqhs,bqh->bqs', jax.nn.relu(logits), wb).astype(jnp.float32)
        causal = key_pos[None, :] <= qpos[:, None]
        score = jnp.where(causal[None], score, -jnp.inf)
        _, idx = lax.top_k(score, n_sel)
        k_sel = jax.vmap(lambda kb, ib: kb[ib])(k, idx)
        v_sel = jax.vmap(lambda vb, ib: vb[ib])(v, idx)
        s = jnp.einsum('bqhd,bqnhd->bhqn', qb, k_sel).astype(jnp.float32) * (HEAD_DIM ** -0.5)
        valid = (idx <= qpos[None, :, None])[:, None]
        p = jax.nn.softmax(jnp.where(valid, s, -jnp.inf), axis=-1).astype(v.dtype)
        return jnp.einsum('bhqn,bqnhd->bqhd', p, v_sel)

    starts = jnp.arange(nblk) * QUERY_BLOCK
    o = lax.map(block, (to_blocks(q), to_blocks(q_idx), to_blocks(w_idx), starts))
    return jnp.moveaxis(o, 0, 1).reshape(B, S, DSA_W).astype(cols.dtype)


def dilated_branch(q, k, v, window, dilation):
    B, S, H, d = q.shape
    steps = window // dilation
    n = S // dilation
    lb = min(steps, n)
    nb = -(-n // lb)
    pad = nb * lb - n

    def to_sub(t):
        t = t.reshape(B, n, dilation, H, d).transpose(0, 2, 3, 1, 4)
        t = jnp.pad(t, ((0, 0), (0, 0), (0, 0), (0, pad), (0, 0)))
        return t.reshape(B, dilation, H, nb, lb, d)

    def with_prev(t):
        prev = jnp.pad(t, ((0, 0), (0, 0), (0, 0), (1, 0), (0, 0), (0, 0)))[:, :, :, :-1]
        return jnp.concatenate([prev, t], axis=4)

    qs = to_sub(q)
    kk = with_prev(to_sub(k))
    vv = with_prev(to_sub(v)).astype(jnp.float32)
    qi = jnp.arange(lb)[:, None]
    kj = jnp.arange(2 * lb)[None, :]
    dist = lb + qi - kj
    key_idx = jnp.arange(nb)[:, None, None] * lb - lb + kj[None]
    mask = (dist >= 0) & (dist <= steps) & (key_idx >= 0)
    s = jnp.einsum('brhnqd,brhnkd->brhnqk', qs, kk).astype(jnp.float32) * (d ** -0.5)
    s = jnp.where(mask, s, -jnp.inf)
    m = jnp.max(s, axis=-1, keepdims=True)
    p = jnp.exp(s - m)
    den = jnp.sum(p, axis=-1, keepdims=True)
    o = jnp.einsum('brhnqk,brhnkd->brhnqd', p, vv) / den
    lse = (m + jnp.log(den))[..., 0]
    o = o.reshape(B, dilation, H, nb * lb, d)[:, :, :, :n].transpose(0, 3, 1, 2, 4).reshape(B, S, H, d)
    lse = lse.reshape(B, dilation, H, nb * lb)[:, :, :, :n].transpose(0, 3, 1, 2).reshape(B, S, H)
    return o, lse


def dilated_mixer(cols):
    B, S, _ = cols.shape
    q, k, v = jnp.split(cols, 3, axis=-1)
    shp = (B, S, DIL_HEADS, HEAD_DIM)
    q, k, v = q.reshape(shp), k.reshape(shp), v.reshape(shp)
    outs = []
    lses = []
    for window, dilation in DIL_PATTERNS:
        o, lse = dilated_branch(q, k, v, window, dilation)
        outs.append(o)
        lses.append(lse)
    alpha = jax.nn.softmax(jnp.stack(lses, axis=0), axis=0)
    o = jnp.sum(alpha[..., None] * jnp.stack(outs, axis=0), axis=0)
    return o.reshape(B, S, DIL_W).astype(cols.dtype)


def hybrid_layer(x, attn_norm_w, w_in, ret_norm_w, kv_norm_w, w_uk, w_uv, w_out, mlp_norm_w, w_up, w_down, pos):
    h = rms_norm(x, attn_norm_w)
    proj = h @ w_in
    ret_cols, dsa_cols, dil_cols = jnp.split(proj, _offsets((RET_COLS, DSA_COLS, DIL_COLS)), axis=-1)
    mixed = jnp.concatenate([retention_mixer(ret_cols, ret_norm_w, pos),
                             dsa_mixer(dsa_cols, kv_norm_w, w_uk, w_uv),
                             dilated_mixer(dil_cols)], axis=-1)
    x = x + mixed @ w_out
    h = rms_norm(x, mlp_norm_w)
    return x + jnp.square(jax.nn.relu(h @ w_up)) @ w_down


def setup_inputs(seed: int = 0) -> dict:
    key = jax.random.key(seed)
    ks = jax.random.split(key, 12)
    f32 = jnp.float32

    def nrm(k, shape, scale):
        return jax.random.normal(k, shape, f32) * scale

    return {
        'x': nrm(ks[0], (BATCH, SEQ, D_MODEL), 1.0),
        'attn_norm_w': 1.0 + nrm(ks[1], (DEPTH, D_MODEL), 0.02),
        'w_in': nrm(ks[2], (DEPTH, D_MODEL, N_IN), D_MODEL ** -0.5),
        'ret_norm_w': 1.0 + nrm(ks[3], (DEPTH, RET_HEADS, HEAD_DIM), 0.02),
        'dsa_kv_norm_w': 1.0 + nrm(ks[4], (DEPTH, KV_LATENT), 0.02),
        'dsa_w_uk': nrm(ks[5], (DEPTH, KV_LATENT, DSA_W), KV_LATENT ** -0.5),
        'dsa_w_uv': nrm(ks[6], (DEPTH, KV_LATENT, DSA_W), KV_LATENT ** -0.5),
        'w_out': nrm(ks[7], (DEPTH, D_MIX, D_MODEL), D_MIX ** -0.5),
        'mlp_norm_w': 1.0 + nrm(ks[8], (DEPTH, D_MODEL), 0.02),
        'w_up': nrm(ks[9], (DEPTH, D_MODEL, D_FF), D_MODEL ** -0.5),
        'w_down': nrm(ks[10], (DEPTH, D_FF, D_MODEL), D_FF ** -0.5),
        'final_norm_w': 1.0 + nrm(ks[11], (D_MODEL,), 0.02),
    }


def reference(x, attn_norm_w, w_in, ret_norm_w, dsa_kv_norm_w, dsa_w_uk, dsa_w_uv, w_out, mlp_norm_w, w_up, w_down, final_norm_w):
    pos = jnp.arange(x.shape[1])
    for layer in range(DEPTH):
        x = hybrid_layer(x, attn_norm_w[layer], w_in[layer], ret_norm_w[layer], dsa_kv_norm_w[layer],
                         dsa_w_uk[layer], dsa_w_uv[layer], w_out[layer], mlp_norm_w[layer],
                         w_up[layer], w_down[layer], pos)
    return rms_norm(x, final_norm_w)
```

```python
import os
import numpy as np
import ml_dtypes
import concourse.bass as bass
import concourse.mybir as mybir
from concourse.bass_utils import run_bass_kernel_spmd

F32 = mybir.dt.float32
BF16 = mybir.dt.bfloat16
ALU = mybir.AluOpType
AF = mybir.ActivationFunctionType
AX = mybir.AxisListType

D = 1024
NWIN = 3712
NBIS = 20
NEG = -1.0e30
NEGB = -30000.0
EPS = 1e-6
R_KI, R_KS, R_KD, R_VS, R_VD, R_KV, ROWS = 0, 128, 384, 896, 1156, 1676, 1804


class Ev:
    __slots__ = ("sem", "val")

    def __init__(self, sem, val):
        self.sem = sem
        self.val = val


class Buf:
    def __init__(self, name="b"):
        self.name = name
        self.w = None
        self.rs = {}


class TB(Buf):
    def __init__(self, name, t):
        super().__init__(name)
        self.t = t

    def __getitem__(self, idx):
        return self.t[idx]


class Ctx:
    def __init__(self, nc):
        self.nc = nc
        self.eng = {"pe": nc.tensor, "act": nc.scalar, "dve": nc.vector, "pool": nc.gpsimd, "sp": nc.sync}
        self.sem = {}
        self.cnt = {}
        self.nsem = 0
        for e in ("pe", "act", "dve", "pool"):
            self._newsem(e)
        self.dsem = {}
        self.dcnt = {}
        self.waited = {e: {} for e in self.eng}
        self.all_dsems = []
        self.free_dsems = []
        self.scope_dsems = []
        self.ntens = 0
        self.stack = []

    def _newsem(self, e):
        s = self.nc.semaphore(f"s_{e}_{self.nsem}").__enter__()
        self.nsem += 1
        self.sem[e] = s
        self.cnt[e] = 0

    def _wait(self, e, ev):
        if ev is None:
            return
        if e == "pe" and ev.sem is self.sem.get("pe"):
            return
        k = id(ev.sem)
        if self.waited[e].get(k, 0) >= ev.val:
            return
        self.eng[e].wait_ge(ev.sem, ev.val)
        self.waited[e][k] = ev.val

    def _deps(self, e, reads, writes):
        for b in reads:
            self._wait(e, b.w)
        for b in writes:
            self._wait(e, b.w)
            for ev in b.rs.values():
                self._wait(e, ev)

    def _record(self, ev, reads, writes):
        for b in reads:
            b.rs[id(ev.sem)] = ev
        for b in writes:
            b.w = ev
            b.rs = {}

    def op(self, e, fn, reads=(), writes=()):
        self._deps(e, reads, writes)
        ins = fn()
        if self.cnt[e] >= 30000:
            self._newsem(e)
        self.cnt[e] += 1
        ins.then_inc(self.sem[e], 1)
        ev = Ev(self.sem[e], self.cnt[e])
        self._record(ev, reads, writes)
        return ev

    def dma(self, q, out, in_, key, reads=(), writes=()):
        self._deps(q, reads, writes)
        buf = (list(writes) + list(reads))[0]
        if getattr(buf, "dsem", None) is None:
            fl = [d for d in self.free_dsems if d[2] == q]
            if fl:
                buf.dsem = fl[0]
                self.free_dsems.remove(fl[0])
            else:
                buf.dsem = [self.nc.semaphore(f"d_{q}_{self.nsem}").__enter__(), 0, q]
                self.nsem += 1
                self.all_dsems.append(buf.dsem)
            self.scope_dsems[-1].append(buf.dsem)
        assert buf.dsem[2] == q, (buf.name, q)
        ins = self.eng[q].dma_start(out=out, in_=in_)
        buf.dsem[1] += 16
        ins.then_inc(buf.dsem[0], 16)
        ev = Ev(buf.dsem[0], buf.dsem[1])
        self._record(ev, reads, writes)
        return ev

    def barrier(self):
        evs = [Ev(self.sem[e], self.cnt[e]) for e in self.sem if self.cnt[e] > 0]
        evs += [Ev(d[0], d[1]) for d in self.all_dsems if d[1] > 0]
        evs += [Ev(self.dsem[k], self.dcnt[k]) for k in self.dsem if self.dcnt[k] > 0]
        for e in self.eng:
            for ev in evs:
                self._wait(e, ev)

    def push(self):
        self.stack.append([])
        self.scope_dsems.append([])

    def pop(self):
        self.free_dsems.extend(self.scope_dsems.pop())
        for g in reversed(self.stack.pop()):
            g.__exit__(None, None, None)

    def sb(self, name, shape, dt):
        g = self.nc.sbuf_tensor(f"{name}_{self.ntens}", list(shape), dt)
        self.ntens += 1
        t = g.__enter__()
        self.stack[-1].append(g)
        return TB(name, t)

    def ps(self, name, shape, dt):
        g = self.nc.psum_tensor(f"{name}_{self.ntens}", list(shape), dt)
        self.ntens += 1
        t = g.__enter__()
        self.stack[-1].append(g)
        return TB(name, t)


def build(S, L):
    TL = S // 4
    NB = TL // 128
    TT = min(512, TL)
    NT = TL // TT
    SBK = TT // 128
    nc = bass.Bass("TRN2", target_bir_lowering=False)
    C = Ctx(nc)
    op, dma = C.op, C.dma
    V, A, PE, G = nc.vector, nc.scalar, nc.tensor, nc.gpsimd

    def din(name, shape, dt=F32):
        return nc.dram_tensor(name, list(shape), dt, kind="ExternalInput").ap()

    xT_d = din("xT", [D, TL])
    w_in = din("w_in", [L, D, NWIN])
    w_out = din("w_out", [L, D, D])
    w_up = din("w_up", [L, D, 4 * D])
    w_down = din("w_down", [L, 4 * D, D])
    w_uk = din("w_uk", [L, 128, 256])
    w_uv = din("w_uv", [L, 128, 256])
    NF = 16 * L + 8 + L + 256 + 256 + 9 * 128 + 32
    cf_d = din("cf32", [128, NF])
    NBF = 128 * 3 + 512 + 512 + 20 * 128 + 8 * 128
    cb_d = din("cbf", [128, NBF], BF16)
    rnw_d = din("rnw", [L, 128, 256])
    cos_d = din("cos_t", [TL, 256])
    sin_d = din("sin_t", [TL, 256])
    outT = nc.dram_tensor("outT", [D, TL], F32, kind="ExternalOutput").ap()
    pieces = [("KI", 128, TL)] + [(f"KS{a}", 128, TL) for a in range(2)] + [(f"KD{a}", 128, TL) for a in range(4)] \
        + [(f"VS{t}", TT, 260) for t in range(NT)] + [(f"VD{t}", TT, 520) for t in range(NT)] + [("KV", NB * 128, 128)]
    GIt = {nm: nc.dram_tensor(f"GI_{nm}", [r_, c_], BF16) for nm, r_, c_ in pieces}
    GOt = [{nm: nc.dram_tensor(f"GO{i}_{nm}", [4 * r_, c_], BF16) for nm, r_, c_ in pieces} for i in range(2)]
    GIp = {nm: GIt[nm].ap() for nm in GIt}
    QD = nc.dram_tensor("QD", [512, TL], BF16).ap()
    QS = nc.dram_tensor("QS", [256, TL], BF16).ap()
    QI = nc.dram_tensor("QI", [512, TL], BF16).ap()
    RQT = nc.dram_tensor("RQT", [256, TL], BF16).ap()
    RKT = nc.dram_tensor("RKT", [256, TL], BF16).ap()
    RV = nc.dram_tensor("RV", [TL, 256], BF16).ap()
    RG = nc.dram_tensor("RG", [TL, 256], F32).ap()

    def flat_rows(ap2d, r0, nrows, f):
        return ap2d[r0:r0 + nrows, :].rearrange("r c -> (r c)").rearrange("(t f) -> t f", f=f)

    C.push()
    xT = C.sb("xT", [128, 8, TL], F32)
    XB = [Buf(f"x{j}") for j in range(NB)]
    cf = C.sb("cf", [128, NF], F32)
    cb = C.sb("cb", [128, NBF], BF16)
    o = 0
    anw = cf[:, o:o + 8 * L]; o += 8 * L
    mnw = cf[:, o:o + 8 * L]; o += 8 * L
    fnw = cf[:, o:o + 8]; o += 8
    kvw = cf[:, o:o + L]; o += L
    kscale = cf[:, o:o + 256]; o += 256
    xi_t = cf[:, o:o + 256]; o += 256
    coef = cf[:, o:o + 9 * 128]; o += 9 * 128
    pow2 = cf[:, o:o + 32]; o += 32
    o = 0
    ident = cb[:, o:o + 128]; o += 128
    negI = cb[:, o:o + 128]; o += 128
    ones = cb[:, o:o + 128]; o += 128
    tri = cb[:, o:o + 512]; o += 512
    cbias = cb[:, o:o + 512]; o += 512
    dilhi = cb[:, o:o + 2560]; o += 2560
    dillo = cb[:, o:o + 1024]; o += 1024
    zer = C.sb("zer", [128, 260], BF16)
    WIres = C.sb("WIres", [128, NB, 8], F32)
    PS = [C.ps(f"ps{i}", [128, 512], F32) for i in range(8)]
    ps7b = PS[7].t[:].bitcast(BF16)

    dma("sp", cf[:, :], cf_d[:, :], "ld0", writes=[cf])
    dma("sp", cb[:, :], cb_d[:, :], "ld0", writes=[cb])
    for k in range(8):
        dma("sp", xT[:, k, :], xT_d[k * 128:(k + 1) * 128, :], "ld0", writes=XB)
    op("dve", lambda: V.memset(zer[:, :], 0.0), writes=[zer])

    def rstd_from(ps_ap, out_ap, n, reads, writes):
        op("act", lambda: A.activation(out=out_ap, in_=ps_ap, func=AF.Ln, scale=1.0 / n, bias=EPS), reads, writes)
        op("act", lambda: A.activation(out=out_ap, in_=out_ap, func=AF.Exp, scale=-0.5), writes, writes)

    def norm_to_hT(hT, HB, nw_ap, l, sq, rs):
        for t in range(NT):
            ts = slice(t * TT, (t + 1) * TT)
            xb = XB[t * SBK:(t + 1) * SBK]
            for k in range(8):
                s = sq[k % 2]
                op("act", lambda: A.activation(out=s[:, :TT], in_=xT[:, k, ts], func=AF.Square), xb, [s])
                op("pe", lambda: PE.matmul(PS[7][:, :TT], lhsT=ones, rhs=s[:, :TT], start=(k == 0), stop=(k == 7)),
                   [s, cb], [PS[7]])
            rstd_from(PS[7][:, :TT], rs[:, :TT], D, [PS[7]], [rs])
            for k in range(8):
                op("dve", lambda: V.scalar_tensor_tensor(out=hT[:, k, ts], in0=xT[:, k, ts],
                                                         scalar=nw_ap[:, l * 8 + k:l * 8 + k + 1], in1=rs[:, :TT],
                                                         op0=ALU.mult, op1=ALU.mult), xb + [rs, cf], [HB[t]])

    def phase_A(l):
        C.push()
        NDB = int(os.environ.get('KNDB', '2'))
        hT = C.sb("hT", [128, 8, TL], BF16)
        HB = [Buf(f"h{t}") for t in range(NT)]
        wb = [C.sb("wA", [128, 8, 512], BF16) for _ in range(2)]
        wuk = C.sb("wuk", [128, 256], BF16)
        wuv = C.sb("wuv", [128, 256], BF16)
        sq = [C.sb("sq", [128, TT], BF16) for _ in range(2)]
        rs = C.sb("rs", [128, TT], F32)
        RKres = C.sb("RKres", [128, NB, 256], BF16)
        RKB = [Buf() for _ in range(NB)]
        stg = [C.sb("stg", [128, TT], BF16) for _ in range(4)]
        ckvn = C.sb("ckvn", [128, TT], BF16)
        vst = [C.sb("vst", [128, 4, 65], BF16) for _ in range(NDB)]
        vdt = [C.sb("vdt", [128, 8, 65], BF16) for _ in range(NDB)]
        cst = [C.sb("cst", [128, 256], F32) for _ in range(NDB)]
        snt = [C.sb("snt", [128, 256], F32) for _ in range(NDB)]
        tA = C.sb("tA", [128, 256], F32)
        tB = C.sb("tB", [128, 256], F32)
        qk = C.sb("qk", [128, 512], F32)
        qb = [C.sb("qb", [128, 256], BF16) for _ in range(NDB)]
        trs = [C.sb("trs", [128, 4, 128], BF16) for _ in range(NDB)]
        vb = [C.sb("vb", [128, 256], BF16) for _ in range(NDB)]
        gf = [C.sb("gf", [128, 256], F32) for _ in range(NDB)]
        kvt = [C.sb("kvt", [128, 256], BF16) for _ in range(NDB)]
        for v_ in vst + vdt:
            op("dve", lambda: V.memset(v_[:, :, 64:65], 1.0), writes=[v_])

        groups = [(0, 512, "fm"), (512, 512, "fm"), (1024, 512, "fm"), (1536, 512, "fm"),
                  (2048, 512, "tm"), (2560, 512, "tm"), (3072, 512, "tm"), (3584, 128, "tm")]

        def load_w(g):
            c0, ncol, _ = groups[g]
            dma("pool", wb[g % 2][:, :, 0:ncol], w_in[l, :, c0:c0 + ncol].rearrange("(k p) n -> p k n", p=128),
                "ldw", writes=[wb[g % 2]])

        load_w(0)
        dma("pool", wuk[:, :], w_uk[l, :, :], "ldw", writes=[wuk])
        dma("pool", wuv[:, :], w_uv[l, :, :], "ldw", writes=[wuv])
        norm_to_hT(hT, HB, anw, l, sq, rs)
        psi = [0]
        sti = [0]

        def nps():
            psi[0] += 1
            return PS[psi[0] % 4]

        def nstg():
            sti[0] += 1
            return stg[sti[0] % 4]

        NGRP = int(os.environ.get('KSTOPG', '8'))
        for g in range(NGRP):
            if g + 1 < 8:
                load_w(g + 1)
            W = wb[g % 2]
            c0, ncol, kind = groups[g]
            for t in range(NT):
                ts = slice(t * TT, (t + 1) * TT)
                if kind == "fm":
                    for c in range(4):
                        p = nps()
                        for k in range(8):
                            op("pe", lambda: PE.matmul(p[:, :TT], lhsT=W[:, k, c * 128:(c + 1) * 128], rhs=hT[:, k, ts],
                                                       start=(k == 0), stop=(k == 7)), [W, HB[t]], [p])
                        if g == 2 and c == 2:
                            s = sq[0]
                            op("act", lambda: A.activation(out=s[:, :TT], in_=p[:, :TT], func=AF.Square), [p], [s])
                            op("pe", lambda: PE.matmul(PS[4][:, :TT], lhsT=ones, rhs=s[:, :TT], start=True, stop=True),
                               [s, cb], [PS[4]])
                            rstd_from(PS[4][:, :TT], rs[:, :TT], 128, [PS[4]], [rs])
                            op("dve", lambda: V.scalar_tensor_tensor(out=ckvn[:, :TT], in0=p[:, :TT],
                                                                     scalar=kvw[:, l:l + 1], in1=rs[:, :TT],
                                                                     op0=ALU.mult, op1=ALU.mult), [p, rs, cf], [ckvn])
                            for c2 in range(2):
                                op("pe", lambda: PE.matmul(PS[5][:, :TT], lhsT=wuk[:, c2 * 128:(c2 + 1) * 128],
                                                           rhs=ckvn[:, :TT], start=True, stop=True), [wuk, ckvn], [PS[5]])
                                st = nstg()
                                op("act", lambda: A.activation(out=st[:, :TT], in_=PS[5][:, :TT], func=AF.Copy), [PS[5]], [st])
                                dma("sp", GIp[f"KS{c2}"][:, ts], st[:, :TT], "stA", reads=[st])
                            for s_ in range(SBK):
                                blk = t * SBK + s_
                                op("pe", lambda: PE.matmul(PS[6][:, 0:256], lhsT=ckvn[:, s_ * 128:(s_ + 1) * 128],
                                                           rhs=wuv[:, :], start=True, stop=True), [wuv, ckvn], [PS[6]])
                                vt = vst[blk % NDB]
                                op("act", lambda: A.activation(out=vt[:, :, 0:64],
                                                               in_=PS[6][:, 0:256].rearrange("p (h e) -> p h e", h=4),
                                                               func=AF.Copy), [PS[6]], [vt])
                                dma("sp", GIp[f"VS{t}"][s_ * 128:(s_ + 1) * 128, :],
                                    vt[:, :, :].rearrange("p h e -> p (h e)"), "stA", reads=[vt])
                            continue
                        st = nstg()
                        scale = 0.125 if (g == 0 or (g == 2 and c < 2)) else 1.0
                        op("act", lambda: A.activation(out=st[:, :TT], in_=p[:, :TT], func=AF.Copy, scale=scale), [p], [st])
                        if g == 0:
                            dst = QD[c * 128:(c + 1) * 128, ts]
                        elif g == 1:
                            dst = GIp[f"KD{c}"][:, ts]
                        elif g == 2 and c < 2:
                            dst = QS[c * 128:(c + 1) * 128, ts]
                        elif g == 2:
                            dst = GIp["KI"][:, ts]
                        else:
                            dst = QI[c * 128:(c + 1) * 128, ts]
                        dma("sp", dst, st[:, :TT], "stA", reads=[st])
                else:
                    for s_ in range(SBK):
                        blk = t * SBK + s_
                        bs = slice(blk * 128, (blk + 1) * 128)
                        p = nps()
                        for k in range(8):
                            op("pe", lambda: PE.matmul(p[:, :ncol], lhsT=hT[:, k, bs], rhs=W[:, k, 0:ncol],
                                                       start=(k == 0), stop=(k == 7)), [W, HB[t]], [p])
                        G4S = int(os.environ.get('KG4', '9'))
                        if g == 4:
                            ct, sn = cst[blk % NDB], snt[blk % NDB]
                            dma("sp", ct[:, :], cos_d[bs, :], "ldA", writes=[ct])
                            dma("sp", sn[:, :], sin_d[bs, :], "ldA", writes=[sn])
                            pv = p[:, 0:512].rearrange("p (h t d) -> p h t d", h=8, t=2)
                            qv = qk[:, :].rearrange("p (h t d) -> p h t d", h=8, t=2)
                            c3 = ct[:, :].rearrange("p (h d) -> p h d", h=8)
                            s3 = sn[:, :].rearrange("p (h d) -> p h d", h=8)
                            a3 = tA[:, :].rearrange("p (h d) -> p h d", h=8)
                            b3 = tB[:, :].rearrange("p (h d) -> p h d", h=8)
                            op("dve", lambda: V.tensor_tensor(out=a3, in0=pv[:, :, 0, :], in1=c3, op=ALU.mult), [p, ct], [tA])
                            op("dve", lambda: V.tensor_tensor(out=b3, in0=pv[:, :, 1, :], in1=s3, op=ALU.mult), [p, sn], [tB])
                            op("dve", lambda: V.tensor_tensor(out=qv[:, :, 0, :], in0=a3, in1=b3, op=ALU.subtract), [tA, tB], [qk])
                            op("dve", lambda: V.tensor_tensor(out=a3, in0=pv[:, :, 1, :], in1=c3, op=ALU.mult), [p, ct], [tA])
                            op("dve", lambda: V.tensor_tensor(out=b3, in0=pv[:, :, 0, :], in1=s3, op=ALU.mult), [p, sn], [tB])
                            op("dve", lambda: V.tensor_tensor(out=qv[:, :, 1, :], in0=a3, in1=b3, op=ALU.add), [tA, tB], [qk])
                            if G4S < 2:
                                continue
                            q_ = qb[blk % NDB]
                            op("act", lambda: A.activation(out=q_[:, :], in_=qk[:, 0:256], func=AF.Copy), [qk], [q_])
                            op("dve", lambda: V.tensor_tensor(out=RKres[:, blk, :], in0=qk[:, 256:512], in1=kscale,
                                                              op=ALU.mult), [qk, cf], [RKB[blk]])
                            if G4S < 3:
                                continue
                            for a in range(2):
                                op("pe", lambda: PE.transpose(ps7b[:, a * 128:(a + 1) * 128], q_[:, a * 128:(a + 1) * 128], ident),
                                   [q_, cb], [PS[7]])
                            for a in range(2):
                                op("pe", lambda: PE.transpose(ps7b[:, (2 + a) * 128:(3 + a) * 128],
                                                              RKres[:, blk, a * 128:(a + 1) * 128], ident),
                                   [RKB[blk], cb], [PS[7]])
                            tr = trs[blk % NDB]
                            op("act", lambda: A.activation(out=tr[:, :, :].rearrange("p a c -> p (a c)"), in_=ps7b[:, 0:512],
                                                           func=AF.Copy), [PS[7]], [tr])
                            if G4S < 4:
                                continue
                            for a in range(2):
                                dma("sp", RQT[a * 128:(a + 1) * 128, bs], tr[:, a, :], "stA", reads=[tr])
                                dma("sp", RKT[a * 128:(a + 1) * 128, bs], tr[:, 2 + a, :], "stA", reads=[tr])
                        elif g == 5:
                            v_, g_ = vb[blk % NDB], gf[blk % NDB]
                            op("act", lambda: A.activation(out=v_[:, :], in_=p[:, 0:256], func=AF.Copy), [p], [v_])
                            op("act", lambda: A.activation(out=g_[:, :], in_=p[:, 256:512], func=AF.Copy), [p], [g_])
                            G5S = int(os.environ.get('KG5', '9'))
                            if G5S < 2:
                                continue
                            dma("sp", RV[bs, :], v_[:, :], "stA", reads=[v_])
                            if G5S < 3:
                                continue
                            dma("sp", RG[bs, :], g_[:, :], "stA", reads=[g_])
                            if G5S < 4:
                                continue
                            for a in range(2):
                                op("pe", lambda: PE.matmul(PS[5][:, a * 128:(a + 1) * 128], lhsT=RKres[:, blk, a * 128:(a + 1) * 128],
                                                           rhs=v_[:, a * 128:(a + 1) * 128], start=True, stop=True),
                                   [RKB[blk], v_], [PS[5]])
                            kt = kvt[blk % NDB]
                            op("act", lambda: A.activation(out=kt[:, :], in_=PS[5][:, 0:256], func=AF.Copy), [PS[5]], [kt])
                            kvv = GIp["KV"]
                            if G5S < 5:
                                continue
                            for a in range(2):
                                for e in range(2):
                                    dma("sp", kvv[blk * 128 + e * 64:blk * 128 + (e + 1) * 64, a * 64:(a + 1) * 64],
                                        kt[e * 64:(e + 1) * 64, a * 128 + e * 64:a * 128 + (e + 1) * 64], "stA", reads=[kt])
                        elif g == 6:
                            vt = vdt[blk % NDB]
                            op("act", lambda: A.activation(out=vt[:, :, 0:64],
                                                           in_=p[:, 0:512].rearrange("p (h e) -> p h e", h=8),
                                                           func=AF.Copy), [p], [vt])
                            dma("sp", GIp[f"VD{t}"][s_ * 128:(s_ + 1) * 128, :], vt[:, :, :].rearrange("p h e -> p (h e)"),
                                "stA", reads=[vt])
                        else:
                            op("act", lambda: A.activation(out=WIres[:, blk, :], in_=p[:, 0:8], func=AF.Copy,
                                                           scale=float(8.0 ** -0.5 * 64.0 ** -0.5)), [p], [WIres])
        C.barrier()
        C.pop()

    def all_gather(l):
        got = GOt[l % 2]
        C.barrier()
        key = "cc"
        if key not in C.dsem:
            C.dsem[key] = nc.semaphore("ccsem").__enter__()
            C.dcnt[key] = 0
        for nm, r_, c_ in pieces:
            ins = G.collective_compute("AllGather", ALU.bypass, replica_groups=[[0, 1, 2, 3], [4, 5, 6, 7]],
                                       ins=[GIt[nm].ap().opt()], outs=[got[nm].ap().opt()])
            C.dcnt[key] += 1
            ins.then_inc(C.dsem[key])
        C.barrier()
        return {nm: got[nm].ap() for nm in got}

    def phase_B(l, go):
        C.push()
        NKMAX = NB * 128
        wo = C.sb("wo", [128, 8, D], BF16)
        dma("pool", wo[:, :, :], w_out[l, :, :].rearrange("(k p) n -> p k n", p=128), "ldw", writes=[wo])
        rnw = C.sb("rnw", [128, 256], F32)
        dma("sp", rnw[:, :], rnw_d[l, :, :], "ldq", writes=[rnw])
        KI2 = C.sb("KI2", [128, 4, NKMAX], BF16)
        KIB = [Buf() for _ in range(4)]
        SC = C.sb("SC", [128, 4, NKMAX], F32)
        NSL = 2
        SLW = 5376
        STR = C.sb("STR", [128, NSL * SLW], BF16)
        SLB = [Buf(f"sl{i}") for i in range(NSL)]
        QIM = [C.sb("QIM", [128, 8, 128], BF16) for _ in range(2)]
        QSM = [C.sb("QSM", [128, 4, 128], BF16) for _ in range(2)]
        QDM = [C.sb("QDM", [128, 8, 128], BF16) for _ in range(2)]
        RQM = [C.sb("RQM", [128, 4, 128], BF16) for _ in range(2)]
        RKt = [C.sb("RKt", [128, 2, 128], BF16) for _ in range(2)]
        RVt = [C.sb("RVt", [128, 256], BF16) for _ in range(2)]
        RGt = [C.sb("RGt", [128, 256], F32) for _ in range(2)]
        KVg = [C.sb("KVg", [128, 4, 128], BF16) for _ in range(2)]
        DH = C.sb("DH", [128, 8, 128], BF16)
        Rb = [C.sb("Rb", [128, 512], BF16) for _ in range(3)]
        PT = [C.sb("PT", [128, 4, 128], BF16) for _ in range(2)]
        NSt = [C.sb("NSt", [128, 128], BF16) for _ in range(2)]
        mixed = C.sb("mixed", [128, D], BF16)
        mixT = C.sb("mixT", [128, 8, 128], BF16)
        Tst = C.sb("Tst", [128, 128], F32)
        Sf = C.sb("Sf", [128, 128], F32)
        Sb = C.sb("Sb", [128, 128], BF16)
        tm1 = C.sb("tm1", [128, 128], F32)
        AT = C.sb("AT", [128, 4, 128], BF16)
        of = C.sb("of", [128, 4, 64], F32)
        oc = C.sb("oc", [128, 4, 64], F32)
        sg = C.sb("sg", [128, 256], F32)
        st4 = C.sb("st4", [128, 16], F32)
        sel = C.sb("sel", [128, 8], F32)
        Wt = C.sb("Wt", [128, 32], F32)
        rden = C.sb("rden", [128, 8], F32)
        for q_ in QIM + QSM + QDM + RQM:
            op("dve", lambda: V.memset(q_[:, :, :], 0.0), writes=[q_])
        op("dve", lambda: V.memset(Tst[:, :], 0.0), writes=[Tst])

        def load_q(j):
            i = j % 2
            bs = slice(j * 128, (j + 1) * 128)

            def masked(dst, src, npair):
                for a in range(npair):
                    for e in range(2):
                        dma("sp", dst[e * 64:(e + 1) * 64, 2 * a + e, :],
                            src[a * 128 + e * 64:a * 128 + (e + 1) * 64, bs], "ldq", writes=[dst])
            masked(QIM[i], QI, 4)
            masked(QSM[i], QS, 2)
            masked(QDM[i], QD, 4)
            masked(RQM[i], RQT, 2)
            for a in range(2):
                dma("sp", RKt[i][:, a, :], RKT[a * 128:(a + 1) * 128, bs], "ldq", writes=[RKt[i]])
            dma("sp", RVt[i][:, :], RV[bs, :], "ldq", writes=[RVt[i]])
            dma("sp", RGt[i][:, :], RG[bs, :], "ldq", writes=[RGt[i]])
            for rho in range(4):
                dma("sp", KI2[:, rho, bs], go["KI"][rho * 128:(rho + 1) * 128, bs], "ldq", writes=[KIB[rho]])
                dma("sp", KVg[i][:, rho, :],
                    go["KV"][rho * NB * 128 + j * 128:rho * NB * 128 + (j + 1) * 128, :], "ldq", writes=[KVg[i]])

        sli = [0]

        def nslot():
            sli[0] += 1
            return sli[0] % NSL

        load_q(0)
        for j in range(min(NB, int(os.environ.get('KNJ', '99')))):
            i = j % 2
            bs = slice(j * 128, (j + 1) * 128)
            n = (j + 1) * 128
            if j + 1 < NB:
                load_q(j + 1)
            kv = KVg[i]
            op("pool", lambda: G.tensor_tensor(out=Sf[:, :], in0=Tst[:, :], in1=coef[:, 0:128], op=ALU.mult), [Tst, cf], [Sf])
            for rho in range(3):
                op("pool", lambda: G.tensor_tensor(out=tm1[:, :], in0=kv[:, rho, :], in1=coef[:, (1 + rho) * 128:(2 + rho) * 128],
                                                   op=ALU.mult), [kv, cf], [tm1])
                op("pool", lambda: G.tensor_tensor(out=Sf[:, :], in0=Sf[:, :], in1=tm1[:, :], op=ALU.add), [Sf, tm1], [Sf])
            op("act", lambda: A.activation(out=Sb[:, :], in_=Sf[:, :], func=AF.Copy), [Sf], [Sb])
            op("pool", lambda: G.tensor_tensor(out=Tst[:, :], in0=Tst[:, :], in1=coef[:, 4 * 128:5 * 128], op=ALU.mult), [Tst, cf], [Tst])
            for rho in range(4):
                op("pool", lambda: G.tensor_tensor(out=tm1[:, :], in0=kv[:, rho, :], in1=coef[:, (5 + rho) * 128:(6 + rho) * 128],
                                                   op=ALU.mult), [kv, cf], [tm1])
                op("pool", lambda: G.tensor_tensor(out=Tst[:, :], in0=Tst[:, :], in1=tm1[:, :], op=ALU.add), [Tst, tm1], [Tst])
            rq, rk, rv, rg = RQM[i], RKt[i], RVt[i], RGt[i]
            for h in range(4):
                op("pe", lambda: PE.matmul(PS[3][:, h * 128:(h + 1) * 128], lhsT=rk[:, h // 2, :], rhs=rq[:, h, :],
                                           start=True, stop=True), [rk, rq], [PS[3]])
            op("dve", lambda: V.tensor_tensor(out=AT[:, :, :].rearrange("p h c -> p (h c)"), in0=PS[3][:, 0:512], in1=tri,
                                              op=ALU.mult), [PS[3], cb], [AT])
            for h in range(4):
                op("pe", lambda: PE.matmul(PS[4][:, h * 64:(h + 1) * 64], lhsT=AT[:, h, :], rhs=rv[:, h * 64:(h + 1) * 64],
                                           start=True, stop=False), [AT, rv], [PS[4]])
                op("pe", lambda: PE.matmul(PS[4][:, h * 64:(h + 1) * 64], lhsT=rq[:, h, :],
                                           rhs=Sb[:, (h // 2) * 64:(h // 2 + 1) * 64],
                                           start=False, stop=True), [rq, Sb], [PS[4]])
            ofl = of[:, :, :].rearrange("p h e -> p (h e)")
            op("dve", lambda: V.tensor_tensor(out=ofl, in0=PS[4][:, 0:256], in1=xi_t, op=ALU.mult), [PS[4], cf], [of])
            op("dve", lambda: V.tensor_reduce(out=st4[:, 0:4], in_=of[:, :, :], axis=AX.X, op=ALU.add), [of], [st4])
            op("dve", lambda: V.tensor_scalar(out=st4[:, 0:4], in0=st4[:, 0:4], scalar1=1.0 / 64, scalar2=None, op0=ALU.mult),
               [st4], [st4])
            for h in range(4):
                op("dve", lambda: V.tensor_scalar(out=oc[:, h, :], in0=of[:, h, :], scalar1=st4[:, h:h + 1], scalar2=None,
                                                  op0=ALU.subtract), [of, st4], [oc])
                op("act", lambda: A.activation(out=of[:, h, :], in_=oc[:, h, :], func=AF.Square,
                                               accum_out=st4[:, 4 + h:5 + h]), [oc], [of, st4])
            rstd_from(st4[:, 4:8], st4[:, 8:12], 64, [st4], [st4])
            op("act", lambda: A.activation(out=sg[:, :], in_=rg[:, :], func=AF.Silu), [rg], [sg])
            for h in range(4):
                op("dve", lambda: V.scalar_tensor_tensor(out=of[:, h, :], in0=oc[:, h, :], scalar=st4[:, 8 + h:9 + h],
                                                         in1=rnw[:, h * 64:(h + 1) * 64], op0=ALU.mult, op1=ALU.mult),
                   [oc, st4, rnw], [of])
            op("dve", lambda: V.tensor_tensor(out=mixed[:, 0:256], in0=ofl, in1=sg[:, :], op=ALU.mult), [of, sg], [mixed])

            qi = QIM[i]
            for h in range(8):
                op("pool", lambda: G.tensor_scalar(out=DH[:, h, :], in0=ident, scalar1=WIres[:, j, h:h + 1], scalar2=None,
                                                   op0=ALU.mult), [cb, WIres], [DH])
            steps = []
            for rho in range(4):
                for g0 in range(0, n, 512):
                    for h in range(8):
                        steps.append((rho, g0, min(512, n - g0), h))

            def emit_lg(s):
                rho, g0, gw, h = steps[s]
                lg = PS[s % 3]
                op("pe", lambda: PE.matmul(lg[:, :gw], lhsT=qi[:, h, :], rhs=KI2[:, rho, g0:g0 + gw], start=True, stop=True),
                   [qi, KIB[rho]], [lg])
            emit_lg(0)
            for s in range(len(steps)):
                rho, g0, gw, h = steps[s]
                if s + 1 < len(steps):
                    emit_lg(s + 1)
                lg = PS[s % 3]
                r_ = Rb[s % 3]
                scp = PS[5 + (s // 8) % 2]
                op("act", lambda: A.activation(out=r_[:, :gw], in_=lg[:, :gw], func=AF.Relu), [lg], [r_])
                op("pe", lambda: PE.matmul(scp[:, :gw], lhsT=DH[:, h, :], rhs=r_[:, :gw], start=(h == 0), stop=(h == 7)),
                   [DH, r_], [scp])
                if h == 7:
                    op("act", lambda: A.activation(out=SC[:, rho, g0:g0 + gw], in_=scp[:, :gw], func=AF.Copy), [scp], [SC])
            scv = SC[:, :, 0:n]
            junk = STR[:, 0:4 * n].rearrange("p (r k) -> p r k", r=4)
            op("dve", lambda: V.tensor_reduce(out=sel[:, 0:1], in_=scv, axis=AX.XY, op=ALU.max, apply_absolute_value=True),
               [SC], [sel])
            op("dve", lambda: V.tensor_scalar(out=sel[:, 0:1], in0=sel[:, 0:1], scalar1=1.0001, scalar2=1e-30,
                                              op0=ALU.mult, op1=ALU.add), [sel], [sel])
            op("dve", lambda: V.tensor_scalar(out=Wt[:, 0:NBIS + 1], in0=pow2[:, 0:NBIS + 1], scalar1=sel[:, 0:1], scalar2=None,
                                              op0=ALU.mult), [sel, cf], [Wt])
            for rho in range(4):
                op("dve", lambda: V.tensor_tensor(out=SC[:, rho, bs], in0=SC[:, rho, bs], in1=cbias[:, rho * 128:(rho + 1) * 128],
                                                  op=ALU.add), [SC, cb], [SC])
            op("dve", lambda: V.memset(sel[:, 1:2], 0.0), writes=[sel])
            for k in range(NBIS):
                op("dve", lambda: V.tensor_scalar(out=junk, in0=scv, scalar1=sel[:, 1:2], scalar2=None, op0=ALU.is_ge,
                                                  op1=ALU.add, accum_out=sel[:, 2:3]), [SC, sel], SLB + [sel])
                op("dve", lambda: V.tensor_scalar(out=sel[:, 3:4], in0=sel[:, 2:3], scalar1=256.0, scalar2=Wt[:, k:k + 1],
                                                  op0=ALU.is_ge, op1=ALU.mult), [sel, Wt], [sel])
                op("dve", lambda: V.scalar_tensor_tensor(out=sel[:, 1:2], in0=sel[:, 1:2], scalar=Wt[:, k + 1:k + 2],
                                                         in1=sel[:, 3:4], op0=ALU.subtract, op1=ALU.add), [sel, Wt], [sel])
            op("dve", lambda: V.tensor_tensor(out=sel[:, 4:5], in0=sel[:, 1:2], in1=Wt[:, NBIS:NBIS + 1], op=ALU.subtract),
               [sel, Wt], [sel])
            qs = QSM[i]
            op("pe", lambda: PE.matmul(PS[3][:, 0:260], lhsT=zer[:, 0:128], rhs=zer[:, 0:260], start=True, stop=False),
               [zer], [PS[3]])
            kbi = 0
            tot = 4 * (j + 1)
            for rho in range(4):
                for b0 in range(0, j + 1, 8):
                    b1 = min(j + 1, b0 + 8)
                    nbk = b1 - b0
                    sl = nslot()
                    ks = STR[:, sl * SLW:sl * SLW + 2 * 1024].rearrange("p (a k) -> p a k", a=2)
                    vs = STR[:, sl * SLW + 2048:sl * SLW + 2048 + 8 * 260].rearrange("p (b f) -> p b f", b=8)
                    for a in range(2):
                        dma("sp", ks[:, a, 0:nbk * 128], go[f"KS{a}"][rho * 128:(rho + 1) * 128, b0 * 128:b1 * 128],
                            "lds", writes=[SLB[sl]])
                    for b_ in range(nbk):
                        bb = b0 + b_
                        dma("sp", vs[:, b_, :],
                            go[f"VS{bb // SBK}"][rho * TT + (bb % SBK) * 128:rho * TT + (bb % SBK + 1) * 128, :],
                            "lds", writes=[SLB[sl]])
                    for kb in range(b0, b1):
                        nt = NSt[kbi % 2]
                        pst = PS[4 + kbi % 2]
                        pt = PT[kbi % 2]
                        op("pool", lambda: G.tensor_scalar(out=nt[:, :], in0=SC[:, rho, kb * 128:(kb + 1) * 128],
                                                           scalar1=sel[:, 4:5], scalar2=None, op0=ALU.is_lt), [SC, sel], [nt])
                        for h in range(4):
                            op("pe", lambda: PE.matmul(pst[:, h * 128:(h + 1) * 128], lhsT=ks[:, h // 2, (kb - b0) * 128:(kb - b0 + 1) * 128],
                                                       rhs=qs[:, h, :], start=True, stop=False), [SLB[sl], qs], [pst])
                            op("pe", lambda: PE.matmul(pst[:, h * 128:(h + 1) * 128], lhsT=nt[:, :], rhs=negI,
                                                       start=False, stop=True), [nt, cb], [pst])
                        op("act", lambda: A.activation(out=pt[:, :, :].rearrange("p h c -> p (h c)"), in_=pst[:, 0:512], func=AF.Exp),
                           [pst], [pt])
                        kbi += 1
                        for h in range(4):
                            op("pe", lambda: PE.matmul(PS[3][:, h * 65:(h + 1) * 65], lhsT=pt[:, h, :],
                                                       rhs=vs[:, kb - b0, h * 65:(h + 1) * 65], start=False, stop=(kbi == tot and h == 3)),
                               [pt, SLB[sl]], [PS[3]])
            p3 = PS[3][:, 0:260].rearrange("p (h e) -> p h e", h=4)
            op("dve", lambda: V.reciprocal(out=rden[:, 0:4].rearrange("p (h o) -> p h o", o=1), in_=p3[:, :, 64:65]), [PS[3]], [rden])
            for h in range(4):
                op("dve", lambda: V.tensor_scalar(out=mixed[:, 256 + h * 64:256 + (h + 1) * 64], in0=p3[:, h, 0:64],
                                                  scalar1=rden[:, h:h + 1], scalar2=None, op0=ALU.mult), [PS[3], rden], [mixed])
            qd = QDM[i]
            for hg in range(2):
                op("pe", lambda: PE.matmul(PS[6 + hg][:, 0:260], lhsT=zer[:, 0:128], rhs=zer[:, 0:260], start=True, stop=False),
                   [zer], [PS[6 + hg]])
            j0 = max(0, j - 4)
            nbk = j - j0 + 1
            cntd = 0
            totd = 4 * nbk
            for rho in range(4):
                sl = nslot()
                kd = STR[:, sl * SLW:sl * SLW + 4 * 640].rearrange("p (a k) -> p a k", a=4)
                vd = STR[:, sl * SLW + 2560:sl * SLW + 2560 + 5 * 520].rearrange("p (b f) -> p b f", b=5)
                for a in range(4):
                    dma("sp", kd[:, a, 0:nbk * 128], go[f"KD{a}"][rho * 128:(rho + 1) * 128, j0 * 128:(j + 1) * 128],
                        "lds", writes=[SLB[sl]])
                for b_ in range(nbk):
                    bb = j0 + b_
                    dma("sp", vd[:, b_, :],
                        go[f"VD{bb // SBK}"][rho * TT + (bb % SBK) * 128:rho * TT + (bb % SBK + 1) * 128, :],
                        "lds", writes=[SLB[sl]])
                for jp in range(j0, j + 1):
                    dj = j - jp
                    idx = rho * 5 + dj
                    cntd += 1
                    for hg in range(2):
                        pst = PS[4 + hg]
                        pt = PT[hg]
                        for hh in range(4):
                            h = hg * 4 + hh
                            op("pe", lambda: PE.matmul(pst[:, hh * 128:(hh + 1) * 128], lhsT=kd[:, h // 2, (jp - j0) * 128:(jp - j0 + 1) * 128],
                                                       rhs=qd[:, h, :], start=True, stop=False), [SLB[sl], qd], [pst])
                            op("pe", lambda: PE.matmul(pst[:, hh * 128:(hh + 1) * 128], lhsT=ident,
                                                       rhs=dilhi[:, idx * 128:(idx + 1) * 128], start=False, stop=(dj >= 2)),
                               [cb], [pst])
                            if dj < 2:
                                op("pe", lambda: PE.matmul(pst[:, hh * 128:(hh + 1) * 128], lhsT=ident,
                                                           rhs=dillo[:, (rho * 2 + dj) * 128:(rho * 2 + dj + 1) * 128],
                                                           start=False, stop=True), [cb], [pst])
                        op("act", lambda: A.activation(out=pt[:, :, :].rearrange("p h c -> p (h c)"), in_=pst[:, 0:512], func=AF.Exp),
                           [pst], [pt])
                        for hh in range(4):
                            h = hg * 4 + hh
                            op("pe", lambda: PE.matmul(PS[6 + hg][:, hh * 65:(hh + 1) * 65], lhsT=pt[:, hh, :],
                                                       rhs=vd[:, jp - j0, h * 65:(h + 1) * 65], start=False, stop=(cntd == totd and hh == 3)),
                               [pt, SLB[sl]], [PS[6 + hg]])
            for hg in range(2):
                p6 = PS[6 + hg][:, 0:260].rearrange("p (h e) -> p h e", h=4)
                op("dve", lambda: V.reciprocal(out=rden[:, 0:4].rearrange("p (h o) -> p h o", o=1), in_=p6[:, :, 64:65]),
                   [PS[6 + hg]], [rden])
                for hh in range(4):
                    h = hg * 4 + hh
                    op("dve", lambda: V.tensor_scalar(out=mixed[:, 512 + h * 64:512 + (h + 1) * 64], in0=p6[:, hh, 0:64],
                                                      scalar1=rden[:, hh:hh + 1], scalar2=None, op0=ALU.mult),
                       [PS[6 + hg], rden], [mixed])
            for k in range(8):
                op("pe", lambda: PE.transpose(ps7b[:, k * 128:(k + 1) * 128], mixed[:, k * 128:(k + 1) * 128], ident),
                   [mixed, cb], [PS[7]])
            op("act", lambda: A.activation(out=mixT[:, :, :].rearrange("p k c -> p (k c)"), in_=ps7b[:, 0:1024], func=AF.Copy),
               [PS[7]], [mixT])
            for half in range(2):
                pb = PS[half]
                for nn in range(4):
                    n_ = half * 4 + nn
                    for k in range(8):
                        op("pe", lambda: PE.matmul(pb[:, nn * 128:(nn + 1) * 128], lhsT=wo[:, k, n_ * 128:(n_ + 1) * 128],
                                                   rhs=mixT[:, k, :], start=(k == 0), stop=(k == 7)), [wo, mixT], [pb])
                xv = xT[:, half * 4:(half + 1) * 4, bs]
                op("dve", lambda: V.tensor_tensor(out=xv, in0=xv, in1=pb[:, 0:512].rearrange("p (n c) -> p n c", n=4), op=ALU.add),
                   [XB[j], pb], [XB[j]])
        C.barrier()
        C.pop()

    def phase_C(l):
        C.push()
        hT = C.sb("h2T", [128, 8, TL], BF16)
        HB = [Buf(f"h{t}") for t in range(NT)]
        sq = [C.sb("sq", [128, TT], BF16) for _ in range(2)]
        rs = C.sb("rs", [128, TT], F32)
        wu = [C.sb("wu", [128, 8, 512], BF16) for _ in range(2)]
        wd = [C.sb("wd", [128, 4, D], BF16) for _ in range(2)]
        aT = C.sb("aT", [128, 4, TL], BF16)
        AB = [[Buf() for _ in range(4)] for _ in range(NT)]
        rl = [C.sb("rl", [128, TT], F32) for _ in range(2)]
        NG = 8

        def load_w(g):
            dma("pool", wu[g % 2][:, :, :], w_up[l, :, g * 512:(g + 1) * 512].rearrange("(k p) n -> p k n", p=128),
                "ldw", writes=[wu[g % 2]])
            dma("pool", wd[g % 2][:, :, :], w_down[l, g * 512:(g + 1) * 512, :].rearrange("(f p) n -> p f n", p=128),
                "ldw", writes=[wd[g % 2]])
        load_w(0)
        norm_to_hT(hT, HB, mnw, l, sq, rs)
        ci = 0
        for g in range(NG):
            if g + 1 < NG:
                load_w(g + 1)
            for t in range(NT):
                ts = slice(t * TT, (t + 1) * TT)
                for f in range(4):
                    p = PS[ci % 4]
                    r_ = rl[ci % 2]
                    ci += 1
                    for k in range(8):
                        op("pe", lambda: PE.matmul(p[:, :TT], lhsT=wu[g % 2][:, k, f * 128:(f + 1) * 128], rhs=hT[:, k, ts],
                                                   start=(k == 0), stop=(k == 7)), [wu[g % 2], HB[t]], [p])
                    op("act", lambda: A.activation(out=r_[:, :TT], in_=p[:, :TT], func=AF.Relu), [p], [r_])
                    op("pool", lambda: G.tensor_tensor(out=aT[:, f, ts], in0=r_[:, :TT], in1=r_[:, :TT], op=ALU.mult),
                       [r_], [AB[t][f]])
                for nn in range(8):
                    p = PS[4 + nn % 3]
                    for f in range(4):
                        op("pe", lambda: PE.matmul(p[:, :TT], lhsT=wd[g % 2][:, f, nn * 128:(nn + 1) * 128], rhs=aT[:, f, ts],
                                                   start=(f == 0), stop=(f == 3)), [wd[g % 2], AB[t][f]], [p])
                    xb = XB[t * SBK:(t + 1) * SBK]
                    op("dve", lambda: V.tensor_tensor(out=xT[:, nn, ts], in0=xT[:, nn, ts], in1=p[:, :TT], op=ALU.add),
                       xb + [p], xb)
        C.barrier()
        C.pop()

    STOP = os.environ.get("KSTOP", "Z")
    for l in range(L):
        if STOP >= "A":
            phase_A(l)
        if STOP >= "B":
            go = all_gather(l)
        if STOP >= "C":
            phase_B(l, go)
        if STOP >= "D":
            phase_C(l)

    C.push()
    sq = [C.sb("sq", [128, TT], BF16) for _ in range(2)]
    rs = C.sb("rs", [128, TT], F32)
    ot = [C.sb("ot", [128, TT], F32) for _ in range(2)]
    for t in range(NT):
        ts = slice(t * TT, (t + 1) * TT)
        xb = XB[t * SBK:(t + 1) * SBK]
        for k in range(8):
            s = sq[k % 2]
            op("act", lambda: A.activation(out=s[:, :TT], in_=xT[:, k, ts], func=AF.Square), xb, [s])
            op("pe", lambda: PE.matmul(PS[7][:, :TT], lhsT=ones, rhs=s[:, :TT], start=(k == 0), stop=(k == 7)), [s, cb], [PS[7]])
        rstd_from(PS[7][:, :TT], rs[:, :TT], D, [PS[7]], [rs])
        for k in range(8):
            o_ = ot[k % 2]
            op("dve", lambda: V.scalar_tensor_tensor(out=o_[:, :TT], in0=xT[:, k, ts], scalar=fnw[:, k:k + 1], in1=rs[:, :TT],
                                                     op0=ALU.mult, op1=ALU.mult), xb + [rs, cf], [o_])
            dma("sp", outT[k * 128:(k + 1) * 128, ts], o_[:, :TT], "sto", reads=[o_])
    C.barrier()
    return nc


def _tables(S, L, r, attn_norm_w, mlp_norm_w, final_norm_w, dsa_kv_norm_w):
    TL = S // 4
    NB = TL // 128
    f32 = np.float32
    g = 1.0 - 2.0 ** (-5.0 - np.arange(4, dtype=np.float64))
    m = np.arange(128, dtype=np.float64)
    cols = []
    cols.append(attn_norm_w.reshape(L, 8, 128).transpose(2, 0, 1).reshape(128, 8 * L))
    cols.append(mlp_norm_w.reshape(L, 8, 128).transpose(2, 0, 1).reshape(128, 8 * L))
    cols.append(final_norm_w.reshape(8, 128).T)
    cols.append(dsa_kv_norm_w.T)
    ksc = (g[None, :] ** (-(m[:, None] + 1.0))) / 8.0
    cols.append(np.repeat(ksc, 64, axis=1))
    xi = g[None, :] ** (m[:, None] + 1.0)
    cols.append(np.repeat(xi, 64, axis=1))
    Gc = g ** 128.0
    head = (2 * np.arange(2)[None, :, None] + (np.arange(128)[:, None, None] // 64)) + 0 * np.arange(64)[None, None, :]
    head = head.reshape(128, 128)

    def tab(pw):
        return np.where(pw >= 0, Gc[head] ** np.maximum(pw, 0), 0.0) if np.isscalar(pw) else None
    coefs = [Gc[head] ** r]
    for rho in range(3):
        coefs.append(Gc[head] ** (r - rho) if rho < r else np.zeros((128, 128)))
    coefs.append(Gc[head] ** 4)
    for rho in range(4):
        coefs.append(Gc[head] ** (4 - rho))
    cols.append(np.concatenate(coefs, axis=1))
    cols.append(np.tile((2.0 ** -np.arange(32, dtype=np.float64))[None, :], (128, 1)))
    cf = np.concatenate([np.asarray(c, dtype=np.float64) for c in cols], axis=1).astype(f32)

    bf = ml_dtypes.bfloat16
    eye = np.eye(128)
    q = np.arange(128)[:, None]
    kk = np.arange(128)[None, :]
    tri = np.tile((kk >= q).astype(np.float64), (1, 4))
    cbias = []
    for rho in range(4):
        if rho < r:
            cbias.append(np.zeros((128, 128)))
        elif rho == r:
            cbias.append(np.where(kk <= q, 0.0, NEG))
        else:
            cbias.append(np.full((128, 128), NEG))
    cbias = np.concatenate(cbias, axis=1)
    hi, lo = [], []
    key = np.arange(128)[:, None]
    qq = np.arange(128)[None, :]
    for rho in range(4):
        for dj in range(5):
            dist = (4 * dj + r - rho) * 128 + qq - key
            mult = ((dist >= 0) & (dist <= 128)).astype(int) + ((dist >= 0) & (dist <= 512) & (dist % 4 == 0)).astype(int) \
                + ((dist >= 0) & (dist <= 2048) & (dist % 16 == 0)).astype(int)
            lm = np.where(mult > 0, np.log(np.maximum(mult, 1).astype(np.float64)), NEGB)
            h_ = lm.astype(bf)
            hi.append(h_)
            if dj < 2:
                lo.append(np.where(mult > 0, lm - h_.astype(np.float64), 0.0).astype(bf))
    cbf = np.concatenate([eye.astype(bf), (NEGB * eye).astype(bf), np.ones((128, 128), bf), tri.astype(bf),
                          cbias.astype(bf)] + hi + lo, axis=1)
    j = np.arange(NB)[:, None]
    pos = ((4 * j + r) * 128 + np.arange(128)[None, :]).reshape(-1)
    inv = (np.float32(10000.0) ** (-np.arange(32, dtype=f32) / np.float32(32))).astype(f32)
    ang = pos.astype(f32)[:, None] * inv[None, :]
    cos = np.tile(np.cos(ang).astype(f32), (1, 8))
    sin = np.tile(np.sin(ang).astype(f32), (1, 8))
    return cf, np.ascontiguousarray(cbf), np.ascontiguousarray(cos), np.ascontiguousarray(sin)


_NC_CACHE = {}


def kernel(x, attn_norm_w, w_in, ret_norm_w, dsa_kv_norm_w, dsa_w_uk, dsa_w_uv, w_out, mlp_norm_w, w_up, w_down,
           final_norm_w):
    f32 = np.float32
    x = np.asarray(x, f32)
    B, S, _ = x.shape
    L = int(np.asarray(w_in).shape[0])
    TL = S // 4
    NB = TL // 128
    w_in = np.asarray(w_in, f32)
    perm = np.concatenate([np.arange(1992, 2504), np.arange(2504, 3016),
                           np.arange(1024, 1280), np.arange(1280, 1408), np.arange(1920, 1984), np.arange(1920, 1984),
                           np.arange(1408, 1920),
                           np.arange(0, 512), np.arange(512, 1024), np.arange(3016, 3528), np.tile(np.arange(1984, 1992), 16)])
    w_in_p = np.ascontiguousarray(w_in[:, :, perm])
    rnw = np.ascontiguousarray(np.broadcast_to(np.asarray(ret_norm_w, f32).reshape(L, 1, 256), (L, 128, 256)))
    shared = {"w_in": w_in_p, "w_out": np.ascontiguousarray(np.asarray(w_out, f32)),
              "w_up": np.ascontiguousarray(np.asarray(w_up, f32)), "w_down": np.ascontiguousarray(np.asarray(w_down, f32)),
              "w_uk": np.ascontiguousarray(np.asarray(dsa_w_uk, f32)), "w_uv": np.ascontiguousarray(np.asarray(dsa_w_uv, f32)),
              "rnw": rnw}
    in_maps = []
    for c in range(8):
        b, r = c // 4, c % 4
        xb = x[b].reshape(S // 512, 4, 128, D)[:, r].reshape(TL, D)
        cf, cbf, cos, sin = _tables(S, L, r, np.asarray(attn_norm_w, f32), np.asarray(mlp_norm_w, f32),
                                    np.asarray(final_norm_w, f32), np.asarray(dsa_kv_norm_w, f32))
        m = dict(shared)
        m.update({"xT": np.ascontiguousarray(xb.T), "cf32": cf, "cbf": cbf, "cos_t": cos, "sin_t": sin})
        in_maps.append(m)
    key = (S, L)
    if key not in _NC_CACHE:
        _NC_CACHE[key] = build(S, L)
    res = run_bass_kernel_spmd(_NC_CACHE[key], in_maps, core_ids=list(range(8)))
    out = np.empty((B, S, D), f32)
    for c in range(8):
        b, r = c // 4, c % 4
        o = np.asarray(res.results[c]["outT"], f32).T.reshape(NB, 128, D)
        out[b].reshape(S // 512, 4, 128, D)[:, r] = o
    return out
```
